# Optimizing a Trainium2 kernel written in Bass

```python
import math
import jax
import jax.numpy as jnp
from jax import lax
import numpy as np

D_MODEL = 1024
BATCH = 1
SEQ = 16384
DEPTH = 1

N_MEM = 256
NORM_EPS = 1e-6
NEG_INF = -1e30

RWKV_HEADS = 8
RWKV_HEAD_DIM = 64
RWKV_WIDTH = RWKV_HEADS * RWKV_HEAD_DIM
DECAY_LORA = 64
ICLR_LORA = 64
GATE_LORA = 128
RWKV_COLS = 3 * RWKV_WIDTH + DECAY_LORA + ICLR_LORA + GATE_LORA
RWKV_SPLITS = [RWKV_WIDTH, 2 * RWKV_WIDTH, 3 * RWKV_WIDTH, 3 * RWKV_WIDTH + DECAY_LORA, 3 * RWKV_WIDTH + DECAY_LORA + ICLR_LORA]
LN_X_EPS = 64e-5
KK_EPS = 1e-12

MOBA_HEADS = 8
MOBA_HEAD_DIM = 64
MOBA_WIDTH = MOBA_HEADS * MOBA_HEAD_DIM
MOBA_BLOCK = 256
MOBA_TOP_BLOCKS = 3
Q_CHUNK = 64

REL_BUCKETS = 32
REL_MAX_DISTANCE = 4096

N_BRANCHES = 2
IN_COLS = RWKV_COLS + 3 * MOBA_WIDTH + N_BRANCHES * D_MODEL

XATTN_HEADS = 4
XATTN_HEAD_DIM = 128
XATTN_WIDTH = XATTN_HEADS * XATTN_HEAD_DIM

N_GROUPS = 4
EXPERTS_PER_GROUP = 8
N_EXPERTS = N_GROUPS * EXPERTS_PER_GROUP
TOP_K_EXPERTS = 2
D_EXPERT = 512
MOE_BLOCK = 128

kernel_name = 'hybrid_rwkv7_moba_hmoe_layer'


def rmsnorm(x, g):
    xf = x.astype(jnp.float32)
    y = xf * lax.rsqrt(jnp.mean(xf * xf, axis=-1, keepdims=True) + NORM_EPS)
    return (y * g.astype(jnp.float32)).astype(x.dtype)


def t5_bucket(dist):
    n = jnp.maximum(dist, 0)
    max_exact = REL_BUCKETS // 2
    nf = jnp.maximum(n, max_exact).astype(jnp.float32)
    large = max_exact + (jnp.log(nf / max_exact) / math.log(REL_MAX_DISTANCE / max_exact)
                         * (REL_BUCKETS - max_exact)).astype(jnp.int32)
    large = jnp.minimum(large, REL_BUCKETS - 1)
    return jnp.where(n < max_exact, n, large)


def wkv7_scan(r, w, k, v, a, b):
    B, T, H, N = r.shape

    def step(S, inp):
        r_t, w_t, k_t, v_t, a_t, b_t = inp
        sa = jnp.einsum('bhvk,bhk->bhv', S, a_t)
        S = S * w_t[:, :, None, :] + sa[..., None] * b_t[:, :, None, :] + v_t[..., None] * k_t[:, :, None, :]
        return S, jnp.einsum('bhvk,bhk->bhv', S, r_t)

    xs = tuple(jnp.moveaxis(t.astype(jnp.float32), 1, 0) for t in (r, w, k, v, a, b))
    S0 = jnp.zeros((B, H, N, N), jnp.float32)
    _, y = lax.scan(step, S0, xs)
    return jnp.moveaxis(y, 0, 1)


def rwkv7_mixer(u, mu, w0, decay_up, iclr_a0, iclr_up, gate_up, k_k, k_a, r_k, ln_w, ln_b):
    B, T, _ = u.shape
    H, N = RWKV_HEADS, RWKV_HEAD_DIM
    u_prev = jnp.pad(u[:, :-1], ((0, 0), (1, 0), (0, 0)))
    u = u + mu * (u_prev - u)
    r, k, v, wd, ad, gd = jnp.split(u, RWKV_SPLITS, axis=-1)
    w_log = -jax.nn.softplus(-(w0 + jnp.tanh(wd) @ decay_up)) - 0.5
    decay = jnp.exp(-jnp.exp(w_log.astype(jnp.float32)))
    a = jax.nn.sigmoid(iclr_a0 + ad @ iclr_up)
    g = jax.nn.sigmoid(gd) @ gate_up
    kk = (k * k_k).reshape(B, T, H, N).astype(jnp.float32)
    kk = kk * lax.rsqrt(jnp.sum(kk * kk, axis=-1, keepdims=True) + KK_EPS)
    k = k * (1.0 + (a - 1.0) * k_a)
    r_h = r.reshape(B, T, H, N)
    k_h = k.reshape(B, T, H, N)
    v_h = v.reshape(B, T, H, N)
    a_h = a.reshape(B, T, H, N).astype(jnp.float32)
    y = wkv7_scan(r_h, decay.reshape(B, T, H, N), k_h, v_h, -kk, kk * a_h)
    mean = jnp.mean(y, axis=-1, keepdims=True)
    var = jnp.mean(jnp.square(y - mean), axis=-1, keepdims=True)
    y = ((y - mean) * lax.rsqrt(var + LN_X_EPS)).reshape(B, T, RWKV_WIDTH)
    y = y * ln_w.astype(jnp.float32) + ln_b.astype(jnp.float32)
    bonus = jnp.sum((r_h * k_h * r_k).astype(jnp.float32), axis=-1, keepdims=True) * v_h.astype(jnp.float32)
    y = y + bonus.reshape(B, T, RWKV_WIDTH)
    return (y * g.astype(jnp.float32)).astype(u.dtype)


def moba_attention(q, k, v, rel_bias):
    B, T, H, Dh = q.shape
    nb = -(-T // MOBA_BLOCK)
    pad = nb * MOBA_BLOCK - T
    q = q.transpose(0, 2, 1, 3)
    kb = jnp.pad(k.transpose(0, 2, 1, 3), ((0, 0), (0, 0), (0, pad), (0, 0))).reshape(B, H, nb, MOBA_BLOCK, Dh)
    vb = jnp.pad(v.transpose(0, 2, 1, 3), ((0, 0), (0, 0), (0, pad), (0, 0))).reshape(B, H, nb, MOBA_BLOCK, Dh)
    k_mean = jnp.mean(kb.astype(jnp.float32), axis=3)
    n_sel = min(MOBA_TOP_BLOCKS, nb)
    scale = Dh ** -0.5
    bias_hb = rel_bias.T.astype(jnp.float32)
    b_idx = jnp.arange(B)[:, None, None, None]
    h_idx = jnp.arange(H)[None, :, None, None]
    blk_ids = jnp.arange(nb)
    offs = jnp.arange(MOBA_BLOCK)

    def one_chunk(c):
        start = c * Q_CHUNK
        qc = lax.dynamic_slice_in_dim(q, start, Q_CHUNK, axis=2).astype(jnp.float32)
        q_pos = start + jnp.arange(Q_CHUNK)
        q_blk = start // MOBA_BLOCK
        blk_score = jnp.einsum('bhqd,bhnd->bhqn', qc, k_mean)
        blk_score = jnp.where(blk_ids < q_blk, blk_score, NEG_INF)
        _, sel = lax.top_k(blk_score, n_sel)
        sel_ok = sel < q_blk
        k_sel = kb[b_idx, h_idx, sel].astype(jnp.float32)
        v_sel = vb[b_idx, h_idx, sel].astype(jnp.float32)
        k_pos_sel = sel[..., None] * MOBA_BLOCK + offs
        bias_sel = bias_hb[h_idx[..., None], t5_bucket(q_pos[:, None, None] - k_pos_sel)]
        logit_sel = jnp.einsum('bhqd,bhqskd->bhqsk', qc, k_sel) * scale + bias_sel
        logit_sel = jnp.where(sel_ok[..., None], logit_sel, NEG_INF)
        k_own = lax.dynamic_index_in_dim(kb, q_blk, axis=2, keepdims=False).astype(jnp.float32)
        v_own = lax.dynamic_index_in_dim(vb, q_blk, axis=2, keepdims=False).astype(jnp.float32)
        rel_own = q_pos[:, None] - (q_blk * MOBA_BLOCK + offs)[None, :]
        logit_own = jnp.einsum('bhqd,bhkd->bhqk', qc, k_own) * scale + bias_hb[:, t5_bucket(rel_own)]
        logit_own = jnp.where(rel_own >= 0, logit_own, NEG_INF)
        logits = jnp.concatenate([logit_sel.reshape(B, H, Q_CHUNK, n_sel * MOBA_BLOCK), logit_own], axis=-1)
        p = jax.nn.softmax(logits, axis=-1)
        p_sel = p[..., :n_sel * MOBA_BLOCK].reshape(B, H, Q_CHUNK, n_sel, MOBA_BLOCK)
        p_own = p[..., n_sel * MOBA_BLOCK:]
        out = (jnp.einsum('bhqsk,bhqskd->bhqd', p_sel, v_sel)
               + jnp.einsum('bhqk,bhkd->bhqd', p_own, v_own))
        return out.astype(q.dtype)

    out = lax.map(one_chunk, jnp.arange(T // Q_CHUNK))
    return out.transpose(1, 0, 3, 2, 4).reshape(B, T, H * Dh)


def memory_cross_attention(h, mem_n, w_q, w_kv, w_o):
    B, T, _ = h.shape
    M = mem_n.shape[1]
    q = (h @ w_q).reshape(B, T, XATTN_HEADS, XATTN_HEAD_DIM)
    k, v = jnp.split(mem_n @ w_kv, 2, axis=-1)
    k = k.reshape(B, M, XATTN_HEADS, XATTN_HEAD_DIM)
    v = v.reshape(B, M, XATTN_HEADS, XATTN_HEAD_DIM)
    logits = jnp.einsum('bthd,bmhd->bhtm', q, k).astype(jnp.float32) * (XATTN_HEAD_DIM ** -0.5)
    p = jax.nn.softmax(logits, axis=-1).astype(v.dtype)
    o = jnp.einsum('bhtm,bmhd->bthd', p, v).reshape(B, T, XATTN_WIDTH)
    return o @ w_o


def hierarchical_moe(h, w_router_group, w_router_expert, w_exp_gate, w_exp_up, w_exp_down):
    B, T, D = h.shape
    n_tok = B * T
    xf = h.reshape(n_tok, D)
    g_prob = jax.nn.softmax((xf @ w_router_group).astype(jnp.float32), axis=-1)
    g_top_p, g_sel = lax.top_k(g_prob, 1)
    e_logits = (xf @ w_router_expert).astype(jnp.float32).reshape(n_tok, N_GROUPS, EXPERTS_PER_GROUP)
    e_in_group = e_logits[jnp.arange(n_tok), g_sel[:, 0]]
    top_v, top_i = lax.top_k(e_in_group, TOP_K_EXPERTS)
    gate = jax.nn.softmax(top_v, axis=-1) * g_top_p
    expert = g_sel * EXPERTS_PER_GROUP + top_i
    n_assign = n_tok * TOP_K_EXPERTS
    e_flat = expert.reshape(n_assign)
    tok_flat = jnp.repeat(jnp.arange(n_tok, dtype=jnp.int32), TOP_K_EXPERTS)
    w_flat = gate.reshape(n_assign)
    order = jnp.argsort(e_flat)
    e_s, tok_s, w_s = e_flat[order], tok_flat[order], w_flat[order]
    counts = jnp.bincount(e_flat, length=N_EXPERTS)
    start = jnp.cumsum(counts) - counts
    padded = (counts + MOE_BLOCK - 1) // MOE_BLOCK * MOE_BLOCK
    p_end = jnp.cumsum(padded)
    p_start = p_end - padded
    dest = p_start[e_s] + jnp.arange(n_assign) - start[e_s]
    n_blocks = (n_assign + N_EXPERTS * (MOE_BLOCK - 1) + MOE_BLOCK - 1) // MOE_BLOCK
    cap = n_blocks * MOE_BLOCK
    buf_tok = jnp.zeros((cap,), jnp.int32).at[dest].set(tok_s)
    buf_w = jnp.zeros((cap,), jnp.float32).at[dest].set(w_s)
    block_expert = jnp.minimum(jnp.searchsorted(p_end, jnp.arange(n_blocks) * MOE_BLOCK, side='right'), N_EXPERTS - 1)

    def expert_block(args):
        toks, e = args
        xb = xf[toks]
        hid = jax.nn.silu(xb @ w_exp_gate[e]) * (xb @ w_exp_up[e])
        return hid @ w_exp_down[e]

    y_blocks = lax.map(expert_block, (buf_tok.reshape(n_blocks, MOE_BLOCK), block_expert))
    y = y_blocks.reshape(cap, D) * buf_w[:, None].astype(h.dtype)
    return jax.ops.segment_sum(y, buf_tok, num_segments=n_tok).reshape(B, T, D)


def setup_inputs(seed: int = 0) -> dict:
    key = jax.random.key(seed)
    ks = jax.random.split(key, 32)
    f32 = jnp.float32

    def nrm(k, shape, scale):
        return jax.random.normal(k, shape, f32) * scale

    def gain(k, shape, base=1.0):
        return base + 0.02 * jax.random.normal(k, shape, f32)

    L, D = DEPTH, D_MODEL
    return {
        'x': nrm(ks[0], (BATCH, SEQ, D), 1.0),
        'mem': nrm(ks[1], (BATCH, N_MEM, D), 1.0),
        'rel_bias': nrm(ks[2], (REL_BUCKETS, MOBA_HEADS), 0.5),
        'mem_norm': gain(ks[3], (D,)),
        'norm_mix': gain(ks[4], (L, D)),
        'w_in': nrm(ks[5], (L, D, IN_COLS), D ** -0.5),
        'tshift_mu': jax.random.uniform(ks[6], (L, RWKV_COLS), f32),
        'decay_w0': jax.random.uniform(ks[7], (L, RWKV_WIDTH), f32, minval=-6.0, maxval=1.0),
        'decay_up': nrm(ks[8], (L, DECAY_LORA, RWKV_WIDTH), 0.5 * DECAY_LORA ** -0.5),
        'iclr_a0': nrm(ks[9], (L, RWKV_WIDTH), 0.1),
        'iclr_up': nrm(ks[10], (L, ICLR_LORA, RWKV_WIDTH), ICLR_LORA ** -0.5),
        'gate_up': nrm(ks[11], (L, GATE_LORA, RWKV_WIDTH), GATE_LORA ** -0.5),
        'k_k': gain(ks[12], (L, RWKV_WIDTH), 0.85),
        'k_a': gain(ks[13], (L, RWKV_WIDTH)),
        'r_k': nrm(ks[14], (L, RWKV_HEADS, RWKV_HEAD_DIM), 0.1),
        'ln_x_w': gain(ks[15], (L, RWKV_WIDTH)),
        'ln_x_b': nrm(ks[16], (L, RWKV_WIDTH), 0.02),
        'w_o_rwkv': nrm(ks[17], (L, RWKV_WIDTH, D), RWKV_WIDTH ** -0.5),
        'w_o_moba': nrm(ks[18], (L, MOBA_WIDTH, D), MOBA_WIDTH ** -0.5),
        'w_out': nrm(ks[19], (L, D, D), D ** -0.5),
        'norm_xattn': gain(ks[20], (L, D)),
        'w_q_x': nrm(ks[21], (L, D, XATTN_WIDTH), D ** -0.5),
        'w_kv_x': nrm(ks[22], (L, D, 2 * XATTN_WIDTH), D ** -0.5),
        'w_o_x': nrm(ks[23], (L, XATTN_WIDTH, D), XATTN_WIDTH ** -0.5),
        'norm_ffn': gain(ks[24], (L, D)),
        'w_router_group': nrm(ks[25], (L, D, N_GROUPS), D ** -0.5),
        'w_router_expert': nrm(ks[26], (L, D, N_EXPERTS), D ** -0.5),
        'w_exp_gate': nrm(ks[27], (L, N_EXPERTS, D, D_EXPERT), D ** -0.5),
        'w_exp_up': nrm(ks[28], (L, N_EXPERTS, D, D_EXPERT), D ** -0.5),
        'w_exp_down': nrm(ks[29], (L, N_EXPERTS, D_EXPERT, D), D_EXPERT ** -0.5),
        'norm_final': gain(ks[30], (D,)),
    }


def reference(x, mem, rel_bias, mem_norm, norm_mix, w_in, tshift_mu, decay_w0, decay_up, iclr_a0,
              iclr_up, gate_up, k_k, k_a, r_k, ln_x_w, ln_x_b, w_o_rwkv, w_o_moba, w_out,
              norm_xattn, w_q_x, w_kv_x, w_o_x, norm_ffn, w_router_group, w_router_expert,
              w_exp_gate, w_exp_up, w_exp_down, norm_final):
    B, T, _ = x.shape
    mem_n = rmsnorm(mem, mem_norm)
    for l in range(DEPTH):
        h = rmsnorm(x, norm_mix[l])
        u = h @ w_in[l]
        u_rwkv, u_moba, u_gate = jnp.split(u, [RWKV_COLS, RWKV_COLS + 3 * MOBA_WIDTH], axis=-1)
        o_a = rwkv7_mixer(u_rwkv, tshift_mu[l], decay_w0[l], decay_up[l], iclr_a0[l], iclr_up[l],
                          gate_up[l], k_k[l], k_a[l], r_k[l], ln_x_w[l], ln_x_b[l])
        q, k, v = jnp.split(u_moba, 3, axis=-1)
        o_b = moba_attention(q.reshape(B, T, MOBA_HEADS, MOBA_HEAD_DIM),
                             k.reshape(B, T, MOBA_HEADS, MOBA_HEAD_DIM),
                             v.reshape(B, T, MOBA_HEADS, MOBA_HEAD_DIM), rel_bias)
        gate_a, gate_b = jnp.split(u_gate, N_BRANCHES, axis=-1)
        merged = jax.nn.sigmoid(gate_a) * (o_a @ w_o_rwkv[l]) + jax.nn.sigmoid(gate_b) * (o_b @ w_o_moba[l])
        x = x + merged @ w_out[l]
        x = x + memory_cross_attention(rmsnorm(x, norm_xattn[l]), mem_n, w_q_x[l], w_kv_x[l], w_o_x[l])
        x = x + hierarchical_moe(rmsnorm(x, norm_ffn[l]), w_router_group[l], w_router_expert[l],
                                 w_exp_gate[l], w_exp_up[l], w_exp_down[l])
    return rmsnorm(x, norm_final)
```

```python
import math
from contextlib import ExitStack

import numpy as np
import concourse.bass as bass
import concourse.mybir as mybir
from concourse.bass_utils import run_bass_kernel_spmd

F32 = mybir.dt.float32
BF16 = mybir.dt.bfloat16
ALU = mybir.AluOpType
AF = mybir.ActivationFunctionType
AX = mybir.AxisListType

NCORES = 8
T = 16384
D = 1024
TOWN = T // NCORES


class _Op:
    __slots__ = ("eng", "fn", "dma", "waits", "signal", "tok", "idx", "inc")

    def __init__(self, eng, fn, dma):
        self.eng = eng
        self.fn = fn
        self.dma = dma
        self.waits = []
        self.signal = dma
        self.tok = None
        self.idx = -1
        self.inc = 1


class Prog:
    ENGS = ("pe", "act", "dve", "pool", "sp")
    NDMA = 8
    EPOCH = 20000

    def __init__(self, nc, stack):
        self.nc = nc
        self.stack = stack
        self.ops = {e: [] for e in self.ENGS}
        self.last_writer = {}
        self.readers = {}
        self.n = 0
        self.dma_hist = {e: [] for e in self.ENGS}
        self.alias = {}
        self.keys_by_base = {}

    def add(self, eng, fn, reads=(), writes=(), dma=False, inc=None):
        op = _Op(eng, fn, dma)
        op.idx = self.n
        self.n += 1
        if inc is not None:
            op.inc = inc
        deps = {}
        for k in tuple(reads) + tuple(writes):
            b = k[0] if isinstance(k, tuple) else k
            self.keys_by_base.setdefault(b, set()).add(k)
            for ob in self.alias.get(b, ()):
                for ok in self.keys_by_base.get(ob, ()):
                    w = self.last_writer.get(ok)
                    if w is not None:
                        deps[w.idx] = (w, "WAW")
                    for r in self.readers.get(ok, ()):
                        deps[r.idx] = (r, "WAX")
        for k in reads:
            w = self.last_writer.get(k)
            if w is not None:
                deps[w.idx] = (w, "RAW")
        for k in writes:
            w = self.last_writer.get(k)
            if w is not None and w.idx not in deps:
                deps[w.idx] = (w, "WAW")
            for r in self.readers.get(k, ()):
                if r.idx not in deps:
                    deps[r.idx] = (r, "WAR")
        for w, kind in deps.values():
            if w is op:
                continue
            if w.eng == eng and not w.dma and not dma:
                if eng == "pe":
                    continue
                if kind != "RAW":
                    continue
            w.signal = True
            op.waits.append(w)
        if dma:
            h = self.dma_hist[eng]
            if len(h) >= self.NDMA:
                op.waits.append(h[-self.NDMA])
            h.append(op)
        for k in writes:
            self.last_writer[k] = op
            self.readers[k] = []
        for k in reads:
            self.readers.setdefault(k, []).append(op)
        self.ops[eng].append(op)
        return op

    def pe(self, fn, r=(), w=()):
        return self.add("pe", fn, r, w)

    def act(self, fn, r=(), w=()):
        return self.add("act", fn, r, w)

    def dve(self, fn, r=(), w=()):
        return self.add("dve", fn, r, w)

    def pool(self, fn, r=(), w=()):
        return self.add("pool", fn, r, w)

    def dma(self, eng, out, in_, r=(), w=(), **kw):
        return self.add(eng, lambda e: e.dma_start(out=out, in_=in_, **kw), r, w, dma=True)

    def emit(self):
        nc = self.nc
        st = self.stack
        for e in self.ENGS:
            cnt = 0
            sem = None
            nsem = 0
            dsems = None
            j = 0
            for op in self.ops[e]:
                if op.dma:
                    if dsems is None:
                        dsems = [st.enter_context(nc.semaphore(f"d_{e}_{i}")) for i in range(self.NDMA)]
                    op.tok = (dsems[j % self.NDMA], 16 * (j // self.NDMA + 1))
                    j += 1
                elif op.signal:
                    if sem is None or cnt >= self.EPOCH:
                        sem = st.enter_context(nc.semaphore(f"c_{e}_{nsem}"))
                        nsem += 1
                        cnt = 0
                    cnt += 1
                    op.tok = (sem, cnt)
        final_dma = {}
        for e in self.ENGS:
            for op in self.ops[e]:
                if op.dma:
                    final_dma[id(op.tok[0])] = op.tok
        block = st.enter_context(nc.Block())

        def run_engine(e, h):
            waited = {}
            for op in self.ops[e]:
                for d in op.waits:
                    sem, val = d.tok
                    if waited.get(id(sem), 0) < val:
                        h.wait_ge(sem, val)
                        waited[id(sem)] = val
                ins = op.fn(h)
                if op.tok is not None:
                    ins.then_inc(op.tok[0], 16 if op.dma else 1)
            if e == "sp":
                for sem, val in final_dma.values():
                    if waited.get(id(sem), 0) < val:
                        h.wait_ge(sem, val)

        @block.sync
        def _(h):
            run_engine("sp", h)

        @block.tensor
        def _(h):
            run_engine("pe", h)

        @block.scalar
        def _(h):
            run_engine("act", h)

        @block.vector
        def _(h):
            run_engine("dve", h)

        @block.gpsimd
        def _(h):
            run_engine("pool", h)


class Arena:
    def __init__(self, P, ar, nbytes):
        self.P = P
        self.ar = ar
        self.nbytes = nbytes
        self.top = 0
        self.live = []
        self.dead = []
        self.names = set()

    def alloc(self, name, free_shape, dt, parts=128):
        assert name not in self.names, name
        self.names.add(name)
        esz = 4 if dt == F32 else 2
        n = esz
        for d in free_shape:
            n *= d
        n = (n + 31) // 32 * 32
        off = self.top
        self.top += n
        assert self.top <= self.nbytes, (name, self.top)
        v = self.ar[0:parts, off // 4:(off + n) // 4]
        if dt != F32:
            v = v.bitcast(dt)
        ne = 1
        for d in free_shape:
            ne *= d
        v = v[:, 0:ne]
        if len(free_shape) == 2:
            v = v.rearrange("p (a b) -> p a b", a=free_shape[0])
        elif len(free_shape) == 3:
            v = v.rearrange("p (a b c) -> p a b c", a=free_shape[0], b=free_shape[1])
        olds = [d[0] for d in self.dead if d[1] < off + n and off < d[1] + d[2]]
        if olds:
            self.P.alias[name] = olds
        self.live.append((name, off, n))
        return v

    def mark(self):
        return (self.top, len(self.live))

    def release(self, mark):
        self.dead.extend(self.live[mark[1]:])
        del self.live[mark[1]:]
        self.top = mark[0]


ARENA_BYTES = 188 * 1024


class KB:
    def __init__(self, nc, st):
        self.nc = nc
        self.st = st
        self.P = Prog(nc, st)
        ar = st.enter_context(nc.sbuf_tensor("arena", [128, ARENA_BYTES // 4], F32))
        self.A = Arena(self.P, ar, ARENA_BYTES)
        self.PS = [st.enter_context(nc.psum_tensor(f"psb{i}", [128, 512], F32)) for i in range(8)]
        self.dram = {}

    def din(self, name, shape, dt=F32):
        t = self.nc.dram_tensor(name, list(shape), dt, kind="ExternalInput").ap()
        self.dram[name] = t
        return t

    def dout(self, name, shape, dt=F32):
        t = self.nc.dram_tensor(name, list(shape), dt, kind="ExternalOutput").ap()
        self.dram[name] = t
        return t

    def psf(self, i):
        return self.PS[i][:]

    def psb(self, i):
        return self.PS[i][:].bitcast(BF16)

    def common(self):
        A, P = self.A, self.P
        self.ident_d = self.din("c_ident", [128, 128])
        self.ident = A.alloc("ident", [128], BF16)
        P.dma("pool", self.ident, self.ident_d, w=["ident"])
        self.identf = A.alloc("identf", [128], F32)
        P.dma("sp", self.identf, self.ident_d, w=["identf"])
        self.epsb = A.alloc("epsb", [1], F32)
        P.dve(lambda e: e.memset(self.epsb, 1e-6), w=["epsb"])
        self.ones_bf = A.alloc("ones_bf", [128], BF16)
        P.dve(lambda e: e.memset(self.ones_bf, 1.0), w=["ones_bf"])
        self.ss = A.alloc("ss", [4], F32)
        self.rstd = A.alloc("rstd", [4], F32)
        self.junk = A.alloc("junk", [1024], BF16)
        self.xn = A.alloc("xn", [2, 1024], BF16)
        self.gb = A.alloc("gb", [1024], F32)
        self.nrm = 0

    def load_gain(self, g_d):
        self.P.dma("sp", self.gb, g_d.rearrange("(o d) -> o d", o=1).partition_broadcast(128), w=["gb"])

    def rms_rows(self, x_ap, xkeys, slot):
        P = self.P
        s = slot % 4
        ss, rstd = self.ss[:, s:s + 1], self.rstd[:, s:s + 1]
        P.act(lambda e: e.activation(out=self.junk, in_=x_ap, func=AF.Square, accum_out=ss), r=xkeys, w=["junk", ("ss", s)])
        P.act(lambda e: e.activation(out=rstd, in_=ss, func=AF.Sqrt, scale=1.0 / D, bias=self.epsb[:, 0:1]), r=[("ss", s), "epsb"], w=[("rstd", s)])
        P.dve(lambda e: e.reciprocal(out=rstd, in_=rstd), r=[("rstd", s)], w=[("rstd", s)])
        return rstd, ("rstd", s)

    def rms_T(self, x_ap, xkeys, hT_out, hkeys, bank):
        P = self.P
        i = self.nrm
        self.nrm += 1
        rstd, rk = self.rms_rows(x_ap, xkeys, i)
        xn = self.xn[:, i % 2, :]
        xk = ("xn", i % 2)
        P.dve(lambda e: e.scalar_tensor_tensor(out=xn, in0=x_ap, scalar=rstd, in1=self.gb, op0=ALU.mult, op1=ALU.mult),
              r=list(xkeys) + [rk, "gb"], w=[xk])
        pb = self.psb(bank)

        def tr(e):
            for j in range(8):
                ins = e.transpose(out=pb[:, j * 128:(j + 1) * 128], in_=xn[:, j * 128:(j + 1) * 128], identity=self.ident)
            return ins
        P.pe(tr, r=[xk, "ident"], w=[("ps", bank)])
        src = pb.rearrange("p (j t) -> p j t", j=8)
        if i % 2 == 0:
            P.act(lambda e: e.copy(out=hT_out, in_=src), r=[("ps", bank)], w=hkeys)
        else:
            P.dve(lambda e: e.tensor_copy(out=hT_out, in_=src), r=[("ps", bank)], w=hkeys)


def phase_b(K, G_d, out_d, dbg=None):
    nc, P, A = K.nc, K.P, K.A
    NT = TOWN // 128
    NG = TOWN // 512
    xo = K.din("xo", [TOWN, D])
    w_gate = K.din("w_gate", [D, 2048])
    norm_mix = K.din("norm_mix", [D])
    w_oa = K.din("w_o_rwkv", [512, D])
    w_ob = K.din("w_o_moba", [512, D])
    w_out = K.din("w_out", [D, D])
    norm_x = K.din("norm_xattn", [D])
    w_q = K.din("w_q_x", [D, 512])
    w_kv = K.din("w_kv_x", [D, 1024])
    w_ox = K.din("w_o_x", [512, D])
    mem = K.din("mem", [256, D])
    mem_norm = K.din("mem_norm", [D])
    norm_f = K.din("norm_ffn", [D])
    w_rt = K.din("w_router", [D, 36])
    w_eg = K.din("w_exp_gate", [32, D, 512])
    w_eu = K.din("w_exp_up", [32, D, 512])
    w_ed = K.din("w_exp_down", [32, 512, D])
    norm_fin = K.din("norm_final", [D])

    xres = A.alloc("xres", [NT, 1024], F32)
    for q in range(4):
        P.dma("sp", xres[:, 4 * q:4 * q + 4, :], xo[q * 512:(q + 1) * 512, :].rearrange("(t p) d -> p t d", p=128),
              w=[("xres", t) for t in range(4 * q, 4 * q + 4)])
    base_mark = A.mark()

    Wg = A.alloc("Wg", [8, 2048], BF16)
    Wab = A.alloc("Wab", [8, 1024], BF16)
    Wout = A.alloc("Wout", [8, 1024], BF16)
    hTg = A.alloc("hTg", [8, 512], BF16)
    Gs = A.alloc("Gs", [8, 512], BF16)
    mT = A.alloc("mT", [8, 512], BF16)
    sgA = A.alloc("sgA", [2, 512], F32)
    sgB = A.alloc("sgB", [2, 512], F32)
    m1 = A.alloc("m1", [2, 512], F32)
    m2 = A.alloc("m2", [2, 512], F32)
    K.load_gain(norm_mix)
    P.dma("pool", Wg, w_gate.rearrange("(j p) c -> p j c", p=128), w=["Wg"])
    P.dma("pool", Wab[0:64], w_oa.rearrange("(r p) c -> p r c", p=64), w=["Wab"])
    P.dma("pool", Wab[64:128], w_ob.rearrange("(r p) c -> p r c", p=64), w=["Wab"])
    P.dma("pool", Wout, w_out.rearrange("(j p) c -> p j c", p=128), w=["Wout"])
    for tg in range(NG):
        tsl = slice(tg * 512, (tg + 1) * 512)
        P.dma("sp", Gs, G_d[:, :, tsl].rearrange("r p t -> p r t"), w=["Gs"])
        for tt in range(4):
            t = tg * 4 + tt
            K.rms_T(xres[:, t, :], [("xres", t)], hTg[:, :, tt * 128:(tt + 1) * 128], [("hTg", tt)], bank=(t % 2))
        hk = [("hTg", tt) for tt in range(4)]
        for fc in range(8):
            b0 = 2 + 3 * (fc % 2)
            fsl = slice(fc * 128, (fc + 1) * 128)
            par = fc % 2

            def mmA(e, fsl=fsl, b0=b0):
                for r in range(8):
                    ins = e.matmul(K.psf(b0)[:, :], lhsT=Wab[0:64, r, fsl], rhs=Gs[0:64, r, :], start=(r == 0), stop=(r == 7))
                return ins
            P.pe(mmA, r=["Wab", "Gs"], w=[("ps", b0)])

            def mmB(e, fsl=fsl, b0=b0):
                for r in range(8):
                    ins = e.matmul(K.psf(b0 + 1)[:, :], lhsT=Wab[64:128, r, fsl], rhs=Gs[64:128, r, :], start=(r == 0), stop=(r == 7))
                return ins
            P.pe(mmB, r=["Wab", "Gs"], w=[("ps", b0 + 1)])

            def mmG(e, fsl=fsl, b0=b0, off=0):
                for j in range(8):
                    ins = e.matmul(K.psf(b0 + 2)[:, :], lhsT=Wg[:, j, off + fsl.start:off + fsl.stop], rhs=hTg[:, j, :], start=(j == 0), stop=(j == 7))
                return ins
            P.pe(lambda e, f=mmG: f(e, off=0), r=["Wg"] + hk, w=[("ps", b0 + 2)])
            P.act(lambda e, b0=b0, par=par: e.activation(out=sgA[:, par, :], in_=K.psf(b0 + 2), func=AF.Sigmoid), r=[("ps", b0 + 2)], w=[("sgA", par)])
            P.pe(lambda e, f=mmG: f(e, off=1024), r=["Wg"] + hk, w=[("ps", b0 + 2)])
            P.act(lambda e, b0=b0, par=par: e.activation(out=sgB[:, par, :], in_=K.psf(b0 + 2), func=AF.Sigmoid), r=[("ps", b0 + 2)], w=[("sgB", par)])
            P.dve(lambda e, b0=b0, par=par: e.tensor_tensor(out=m1[:, par, :], in0=K.psf(b0), in1=sgA[:, par, :], op=ALU.mult), r=[("ps", b0), ("sgA", par)], w=[("m1", par)])
            P.dve(lambda e, b0=b0, par=par: e.tensor_tensor(out=m2[:, par, :], in0=K.psf(b0 + 1), in1=sgB[:, par, :], op=ALU.mult), r=[("ps", b0 + 1), ("sgB", par)], w=[("m2", par)])
            P.pool(lambda e, fc=fc, par=par: e.tensor_tensor(out=mT[:, fc, :], in0=m1[:, par, :], in1=m2[:, par, :], op=ALU.add), r=[("m1", par), ("m2", par)], w=[("mT", fc)])
        mk = [("mT", fc) for fc in range(8)]
        for tt in range(4):
            t = tg * 4 + tt
            for n in range(2):
                bk = (tt * 2 + n) % 2

                def mmO(e, tt=tt, n=n, bk=bk):
                    for fc in range(8):
                        ins = e.matmul(K.psf(bk), lhsT=mT[:, fc, tt * 128:(tt + 1) * 128], rhs=Wout[:, fc, n * 512:(n + 1) * 512], start=(fc == 0), stop=(fc == 7))
                    return ins
                P.pe(mmO, r=mk + ["Wout"], w=[("ps", bk)])
                xs = xres[:, t, n * 512:(n + 1) * 512]
                P.dve(lambda e, xs=xs, bk=bk: e.tensor_tensor(out=xs, in0=K.psf(bk), in1=xs, op=ALU.add), r=[("ps", bk), ("xres", t)], w=[("xres", t)])
    if dbg is not None and "x1" in dbg:
        P.dma("sp", dbg["x1"].rearrange("(t p) d -> p t d", p=128), xres, r=[("xres", t) for t in range(NT)])
    A.release(base_mark)

    m0 = A.mark()
    Wq = A.alloc("Wq", [8, 512], BF16)
    Wkv = A.alloc("Wkv", [8, 1024], BF16)
    Wox = A.alloc("Wox", [4, 1024], BF16)
    memx = A.alloc("memx", [2, 1024], F32)
    memT = A.alloc("memT", [8, 256], BF16)
    KT = A.alloc("KT", [4, 256], BF16)
    Vx = A.alloc("Vx", [2, 512], BF16)
    h2T = A.alloc("h2T", [8, 512], BF16)
    QT = A.alloc("QT", [4, 512], BF16)
    PT = A.alloc("PT", [2, 2, 512], BF16)
    rden = A.alloc("rden", [2, 512], F32)
    oxT = A.alloc("oxT", [4, 512], BF16)
    P.dma("pool", Wq, w_q.rearrange("(j p) c -> p j c", p=128), w=["Wq"])
    P.dma("pool", Wkv, w_kv.rearrange("(j p) c -> p j c", p=128), w=["Wkv"])
    P.dma("pool", Wox, w_ox.rearrange("(j p) c -> p j c", p=128), w=["Wox"])
    P.dma("sp", memx, mem.rearrange("(t p) d -> p t d", p=128), w=["memx"])
    K.load_gain(mem_norm)
    for mt in range(2):
        K.rms_T(memx[:, mt, :], ["memx"], memT[:, :, mt * 128:(mt + 1) * 128], [("memT", mt)], bank=mt)
    K.load_gain(norm_x)
    mtk = [("memT", 0), ("memT", 1)]
    for hd in range(4):
        def mmK(e, hd=hd):
            for j in range(8):
                ins = e.matmul(K.psf(2)[:, 0:256], lhsT=Wkv[:, j, hd * 128:(hd + 1) * 128], rhs=memT[:, j, :], start=(j == 0), stop=(j == 7))
            return ins
        P.pe(mmK, r=["Wkv"] + mtk, w=[("ps", 2)])
        P.dve(lambda e, hd=hd: e.tensor_copy(out=KT[:, hd, :], in_=K.psf(2)[:, 0:256]), r=[("ps", 2)], w=[("KT", hd)])
    for mt in range(2):
        def mmV(e, mt=mt):
            for j in range(8):
                ins = e.matmul(K.psf(3), lhsT=memT[:, j, mt * 128:(mt + 1) * 128], rhs=Wkv[:, j, 512:1024], start=(j == 0), stop=(j == 7))
            return ins
        P.pe(mmV, r=["Wkv"] + mtk, w=[("ps", 3)])
        P.dve(lambda e, mt=mt: e.tensor_copy(out=Vx[:, mt, :], in_=K.psf(3)), r=[("ps", 3)], w=[("Vx", mt)])
    sc = 128 ** -0.5
    for tg in range(NG):
        for tt in range(4):
            t = tg * 4 + tt
            K.rms_T(xres[:, t, :], [("xres", t)], h2T[:, :, tt * 128:(tt + 1) * 128], [("h2T", tt)], bank=(t % 2))
        hk = [("h2T", tt) for tt in range(4)]
        for hd in range(4):
            def mmQ(e, hd=hd):
                for j in range(8):
                    ins = e.matmul(K.psf(2), lhsT=Wq[:, j, hd * 128:(hd + 1) * 128], rhs=h2T[:, j, :], start=(j == 0), stop=(j == 7))
                return ins
            P.pe(mmQ, r=["Wq"] + hk, w=[("ps", 2)])
            P.act(lambda e, hd=hd: e.activation(out=QT[:, hd, :], in_=K.psf(2), func=AF.Copy, scale=sc), r=[("ps", 2)], w=[("QT", hd)])
            par = hd % 2
            for mt in range(2):
                P.pe(lambda e, hd=hd, mt=mt: e.matmul(K.psf(3 + mt), lhsT=KT[:, hd, mt * 128:(mt + 1) * 128], rhs=QT[:, hd, :], start=True, stop=True),
                     r=[("KT", hd), ("QT", hd)], w=[("ps", 3 + mt)])
                P.act(lambda e, mt=mt, par=par: e.activation(out=PT[:, par, mt, :], in_=K.psf(3 + mt), func=AF.Exp), r=[("ps", 3 + mt)], w=[("PT", par, mt)])

            def mmPV(e, hd=hd, par=par):
                for mt in range(2):
                    ins = e.matmul(K.psf(5), lhsT=Vx[:, mt, hd * 128:(hd + 1) * 128], rhs=PT[:, par, mt, :], start=(mt == 0), stop=(mt == 1))
                for mt in range(2):
                    ins = e.matmul(K.psf(6), lhsT=K.ones_bf, rhs=PT[:, par, mt, :], start=(mt == 0), stop=(mt == 1))
                return ins
            P.pe(mmPV, r=[("Vx", 0), ("Vx", 1), ("PT", par, 0), ("PT", par, 1), "ones_bf"], w=[("ps", 5), ("ps", 6)])
            P.dve(lambda e, par=par: e.reciprocal(out=rden[:, par, :], in_=K.psf(6)), r=[("ps", 6)], w=[("rden", par)])
            P.dve(lambda e, hd=hd, par=par: e.tensor_tensor(out=oxT[:, hd, :], in0=K.psf(5), in1=rden[:, par, :], op=ALU.mult), r=[("ps", 5), ("rden", par)], w=[("oxT", hd)])
        ok = [("oxT", hd) for hd in range(4)]
        for tt in range(4):
            t = tg * 4 + tt
            for n in range(2):
                bk = (tt * 2 + n) % 2

                def mmO2(e, tt=tt, n=n, bk=bk):
                    for hd in range(4):
                        ins = e.matmul(K.psf(bk), lhsT=oxT[:, hd, tt * 128:(tt + 1) * 128], rhs=Wox[:, hd, n * 512:(n + 1) * 512], start=(hd == 0), stop=(hd == 3))
                    return ins
                P.pe(mmO2, r=ok + ["Wox"], w=[("ps", bk)])
                xs = xres[:, t, n * 512:(n + 1) * 512]
                P.dve(lambda e, xs=xs, bk=bk: e.tensor_tensor(out=xs, in0=K.psf(bk), in1=xs, op=ALU.add), r=[("ps", bk), ("xres", t)], w=[("xres", t)])
    if dbg is not None and "x2" in dbg:
        P.dma("sp", dbg["x2"].rearrange("(t p) d -> p t d", p=128), xres, r=[("xres", t) for t in range(NT)])
    A.release(m0)
    return xres


def phase_b_moe(K, xres, out_d, dbg=None):
    nc, P, A = K.nc, K.P, K.A
    NT = TOWN // 128
    NG = TOWN // 512
    d = K.dram
    norm_f, w_rt, w_eg, w_eu, w_ed, norm_fin = d["norm_ffn"], d["w_router"], d["w_exp_gate"], d["w_exp_up"], d["w_exp_down"], d["norm_final"]
    h3T = A.alloc("h3T", [8, TOWN], BF16)
    xn32 = A.alloc("xn32", [2, 1024], F32)
    h3T32 = A.alloc("h3T32", [8, 128], F32)
    wr32 = A.alloc("wr32", [8, 36], F32)
    rl = A.alloc("rl", [36], F32)
    em = A.alloc("em", [32], F32)
    sm = A.alloc("smallr", [32], F32)
    top8 = A.alloc("top8", [8], F32)
    gwa = A.alloc("gwa", [32], F32)
    gw = A.alloc("gw", [NT, 32], F32)
    Weg = A.alloc("Weg", [2, 8, 512], BF16)
    Weu = A.alloc("Weu", [2, 8, 512], BF16)
    Wed = A.alloc("Wed", [2, 4, 1024], BF16)
    sgb = A.alloc("sgb", [2, 512], BF16)
    hidT = A.alloc("hidT", [2, 4, 512], BF16)
    K.load_gain(norm_f)
    P.dma("sp", wr32, w_rt.rearrange("(j p) c -> p j c", p=128), w=["wr32"])

    def load_expert(e):
        s = e % 2
        P.dma("pool", Weg[:, s], w_eg[e].rearrange("(j p) c -> p j c", p=128), w=[("Weg", s)])
        P.dma("pool", Weu[:, s], w_eu[e].rearrange("(j p) c -> p j c", p=128), w=[("Weu", s)])
        P.dma("pool", Wed[:, s], w_ed[e].rearrange("(j p) c -> p j c", p=128), w=[("Wed", s)])
    load_expert(0)
    load_expert(1)

    for t in range(NT):
        xk = [("xres", t)]
        x_ap = xres[:, t, :]
        rstd, rk = K.rms_rows(x_ap, xk, t)
        s2 = t % 2
        xf = xn32[:, s2, :]
        xb = K.xn[:, s2, :]
        P.dve(lambda e, x_ap=x_ap, rstd=rstd, xf=xf: e.scalar_tensor_tensor(out=xf, in0=x_ap, scalar=rstd, in1=K.gb, op0=ALU.mult, op1=ALU.mult),
              r=xk + [rk, "gb"], w=[("xn32", s2)])
        P.act(lambda e, xf=xf, xb=xb: e.copy(out=xb, in_=xf), r=[("xn32", s2)], w=[("xn", s2)])
        pb = K.psb(s2)

        def tr(e, xb=xb, pb=pb):
            for j in range(8):
                ins = e.transpose(out=pb[:, j * 128:(j + 1) * 128], in_=xb[:, j * 128:(j + 1) * 128], identity=K.ident)
            return ins
        P.pe(tr, r=[("xn", s2), "ident"], w=[("ps", s2)])
        P.act(lambda e, pb=pb, t=t: e.copy(out=h3T[:, :, t * 128:(t + 1) * 128], in_=pb.rearrange("p (j t) -> p j t", j=8)), r=[("ps", s2)], w=[("h3T", t)])
        for half in range(2):
            bk = 2 + half

            def tr32(e, xf=xf, half=half, bk=bk):
                for jj in range(4):
                    j = half * 4 + jj
                    ins = e.transpose(out=K.psf(bk)[:, jj * 128:(jj + 1) * 128], in_=xf[:, j * 128:(j + 1) * 128], identity=K.identf)
                return ins
            P.pe(tr32, r=[("xn32", s2), "identf"], w=[("ps", bk)])
            P.dve(lambda e, half=half, bk=bk: e.tensor_copy(out=h3T32[:, half * 4:half * 4 + 4, :], in_=K.psf(bk).rearrange("p (j t) -> p j t", j=4)),
                  r=[("ps", bk)], w=[("h3T32", half)])

        def mmR(e):
            for j in range(8):
                ins = e.matmul(K.psf(4)[:, 0:36], lhsT=h3T32[:, j, :], rhs=wr32[:, j, :], start=(j == 0), stop=(j == 7))
            return ins
        P.pe(mmR, r=[("h3T32", 0), ("h3T32", 1), "wr32"], w=[("ps", 4)])
        P.dve(lambda e: e.tensor_copy(out=rl, in_=K.psf(4)[:, 0:36]), r=[("ps", 4)], w=["rl"])
        gmax, ngmax, gsum, gp, dd, s1c, s2c, w1, w2 = [sm[:, i:i + 1] for i in range(9)]
        og, pen, eg = sm[:, 12:16], sm[:, 16:20], sm[:, 20:24]
        ch = "rchain"
        P.dve(lambda e: e.tensor_reduce(out=gmax, in_=rl[:, 0:4], axis=AX.X, op=ALU.max), r=["rl"], w=[ch])
        P.dve(lambda e: e.tensor_scalar(out=ngmax, in0=gmax, scalar1=-1.0, scalar2=None, op0=ALU.mult), r=[ch], w=[ch])
        P.act(lambda e: e.activation(out=eg, in_=rl[:, 0:4], func=AF.Exp, bias=ngmax, accum_out=gsum), r=[ch, "rl"], w=[ch])
        P.dve(lambda e: e.reciprocal(out=gp, in_=gsum), r=[ch], w=[ch])
        P.dve(lambda e: e.tensor_scalar(out=og, in0=rl[:, 0:4], scalar1=gmax, scalar2=None, op0=ALU.is_equal), r=[ch, "rl"], w=[ch])
        P.dve(lambda e: e.tensor_scalar(out=pen, in0=og, scalar1=1.0, scalar2=1e30, op0=ALU.subtract, op1=ALU.mult), r=[ch], w=[ch])
        for g in range(4):
            P.dve(lambda e, g=g: e.tensor_scalar(out=em[:, 8 * g:8 * g + 8], in0=rl[:, 4 + 8 * g:12 + 8 * g], scalar1=pen[:, g:g + 1], scalar2=None, op0=ALU.add),
                  r=[ch, "rl"], w=["em"])
        P.dve(lambda e: e.max(out=top8, in_=em), r=["em"], w=["top8"])
        P.dve(lambda e: e.tensor_tensor(out=dd, in0=top8[:, 0:1], in1=top8[:, 1:2], op=ALU.subtract), r=["top8"], w=[ch])
        P.act(lambda e: e.activation(out=s1c, in_=dd, func=AF.Sigmoid), r=[ch], w=[ch])
        P.act(lambda e: e.activation(out=s2c, in_=dd, func=AF.Sigmoid, scale=-1.0), r=[ch], w=[ch])
        P.dve(lambda e: e.tensor_tensor(out=w1, in0=s1c, in1=gp, op=ALU.mult), r=[ch], w=[ch])
        P.dve(lambda e: e.tensor_tensor(out=w2, in0=s2c, in1=gp, op=ALU.mult), r=[ch], w=[ch])
        P.dve(lambda e: e.tensor_scalar(out=gwa, in0=em, scalar1=top8[:, 0:1], scalar2=w1, op0=ALU.is_equal, op1=ALU.mult), r=["em", "top8", ch], w=["gwa"])
        P.dve(lambda e, t=t: e.tensor_scalar(out=gw[:, t, :], in0=em, scalar1=top8[:, 1:2], scalar2=w2, op0=ALU.is_equal, op1=ALU.mult), r=["em", "top8", ch], w=[("gw", t)])
        P.dve(lambda e, t=t: e.tensor_tensor(out=gw[:, t, :], in0=gw[:, t, :], in1=gwa, op=ALU.add), r=[("gw", t), "gwa"], w=[("gw", t)])
    if dbg is not None and "gw" in dbg:
        P.dma("sp", dbg["gw"].rearrange("(t p) e -> p t e", p=128), gw, r=[("gw", t) for t in range(NT)])

    for ex in range(32):
        s = ex % 2
        for tg in range(NG):
            hp = tg % 2
            hk = [("h3T", t) for t in range(tg * 4, tg * 4 + 4)]
            for fc in range(4):
                bg = 2 * (fc % 2)
                fsl = slice(fc * 128, (fc + 1) * 128)

                def mmGU(e, W=Weg, bk=bg, fsl=fsl, s=s, tg=tg):
                    for j in range(8):
                        ins = e.matmul(K.psf(bk), lhsT=W[:, s, j, fsl], rhs=h3T[:, j, tg * 512:(tg + 1) * 512], start=(j == 0), stop=(j == 7))
                    return ins
                P.pe(mmGU, r=[("Weg", s)] + hk, w=[("ps", bg)])
                P.pe(lambda e, f=mmGU, bg=bg: f(e, W=Weu, bk=bg + 1), r=[("Weu", s)] + hk, w=[("ps", bg + 1)])
                sp_ = fc % 2
                P.act(lambda e, bg=bg, sp_=sp_: e.activation(out=sgb[:, sp_, :], in_=K.psf(bg), func=AF.Silu), r=[("ps", bg)], w=[("sgb", sp_)])
                P.dve(lambda e, bg=bg, sp_=sp_, hp=hp, fc=fc: e.tensor_tensor(out=hidT[:, hp, fc, :], in0=K.psf(bg + 1), in1=sgb[:, sp_, :], op=ALU.mult),
                      r=[("ps", bg + 1), ("sgb", sp_)], w=[("hidT", hp, fc)])
            hdk = [("hidT", hp, fc) for fc in range(4)]
            for tt in range(4):
                t = tg * 4 + tt
                for n in range(2):
                    bk = 4 + (tt * 2 + n) % 4

                    def mmD(e, bk=bk, tt=tt, n=n, hp=hp, s=s):
                        for fc in range(4):
                            ins = e.matmul(K.psf(bk), lhsT=hidT[:, hp, fc, tt * 128:(tt + 1) * 128], rhs=Wed[:, s, fc, n * 512:(n + 1) * 512], start=(fc == 0), stop=(fc == 3))
                        return ins
                    P.pe(mmD, r=hdk + [("Wed", s)], w=[("ps", bk)])
                    xs = xres[:, t, n * 512:(n + 1) * 512]
                    P.dve(lambda e, xs=xs, bk=bk, t=t, ex=ex: e.scalar_tensor_tensor(out=xs, in0=K.psf(bk), scalar=gw[:, t, ex:ex + 1], in1=xs, op0=ALU.mult, op1=ALU.add),
                          r=[("ps", bk), ("gw", t), ("xres", t)], w=[("xres", t)])
        if ex + 2 < 32:
            load_expert(ex + 2)
    if dbg is not None and "x3" in dbg:
        P.dma("sp", dbg["x3"].rearrange("(t p) d -> p t d", p=128), xres, r=[("xres", t) for t in range(NT)])

    K.load_gain(norm_fin)
    for t in range(NT):
        xk = [("xres", t)]
        x_ap = xres[:, t, :]
        rstd, rk = K.rms_rows(x_ap, xk, t)
        s2 = t % 2
        xf = xn32[:, s2, :]
        P.dve(lambda e, x_ap=x_ap, rstd=rstd, xf=xf: e.scalar_tensor_tensor(out=xf, in0=x_ap, scalar=rstd, in1=K.gb, op0=ALU.mult, op1=ALU.mult),
              r=xk + [rk, "gb"], w=[("xn32", s2)])
        P.dma("sp", out_d[t * 128:(t + 1) * 128, :], xf, r=[("xn32", s2)])


C0 = math.exp(-0.5)


def phase_a1_rwkv(K, x_d, XI_d, QS_d, KS_d, VS_d, kms_out, dbg=None, ngroups=32, nrw=32):
    nc, P, A = K.nc, K.P, K.A
    wa_d = K.din("wa", [D, 640])
    pvec_d = K.din("pvec", [128, 16])
    dup_d = K.din("decay_up_h", [64, 64])
    iup_d = K.din("iclr_up_h", [64, 64])
    gup_d = K.din("gate_up_h", [128, 64])
    crw_d = K.din("c_rwkv", [64, 5, 512])
    norm_mix = K.din("norm_mix", [D]) if "norm_mix" not in K.dram else K.dram["norm_mix"]

    WA = A.alloc("WA", [8, 640], BF16)
    P.dma("pool", WA, wa_d.rearrange("(j p) c -> p j c", p=128), w=["WA"])
    pv = A.alloc("pv", [16], F32)
    P.dma("sp", pv, pvec_d, w=["pv"])
    pv1 = A.alloc("pv1", [16], F32)
    P.dve(lambda e: e.tensor_scalar(out=pv1, in0=pv, scalar1=-1.0, scalar2=1.0, op0=ALU.mult, op1=ALU.add), r=["pv"], w=["pv1"])
    dup = A.alloc("dup", [64], BF16, parts=64)
    iup = A.alloc("iup", [64], BF16, parts=64)
    gup = A.alloc("gup", [64], BF16)
    P.dma("pool", dup, dup_d, w=["dup"])
    P.dma("pool", iup, iup_d, w=["iup"])
    P.dma("pool", gup, gup_d, w=["gup"])
    msk = A.alloc("msk", [3, 512], BF16, parts=64)
    P.dma("pool", msk, crw_d[:, 0:3, :], w=["msk"])
    cf = A.alloc("cf", [2, 512], F32, parts=64)
    P.dma("sp", cf, crw_d[:, 3:5, :], w=["cf"])
    resetm = cf[:, 0, :]
    identrep = cf[:, 1, :]
    ones64f = A.alloc("ones64f", [64], F32, parts=64)
    P.dve(lambda e: e.memset(ones64f, 1.0), w=["ones64f"])
    eps12 = A.alloc("eps12", [2], F32, parts=64)
    P.dve(lambda e: e.memset(eps12[:, 0:1], 1e-12), w=["eps12"])
    P.dve(lambda e: e.memset(eps12[:, 1:2], 64e-5), w=["eps12"])
    K.load_gain(norm_mix)

    xt = A.alloc("xt", [3, 1024], F32)
    xnT = A.alloc("xnT", [2, 8, 512], BF16)
    U = A.alloc("U", [2, 5, 513], F32, parts=64)
    Ug = A.alloc("Ug", [2, 513], F32)
    P.dve(lambda e: e.memset(U[:, :, :, 0:1], 0.0), w=[("U", 0), ("U", 1)])
    P.dve(lambda e: e.memset(Ug[:, :, 0:1], 0.0), w=[("Ug", 0), ("Ug", 1)])
    mvb = A.alloc("mvb", [512], BF16, parts=64)
    qsb = A.alloc("qsb", [2, 512], BF16, parts=64)
    ksb = A.alloc("ksb", [2, 512], BF16, parts=64)
    vsb = A.alloc("vsb", [2, 4, 64], BF16)
    Hs = A.alloc("Hs", [2, 9, 64], BF16, parts=64)
    P.dve(lambda e: e.memset(Hs[:, 0, 0, :], 0.0), w=[("Hs", 0, 0)])

    def f32t(name):
        return A.alloc(name, [512], F32, parts=64)

    def bft(name, parts=64):
        return A.alloc(name, [512], BF16, parts=parts)
    t1, rm, km, vm = f32t("t1"), f32t("rm"), f32t("km"), f32t("vm")
    tw, adb, sgd = bft("tw"), bft("adb"), bft("sgd", 128)
    sg, Lc, Lex, eL, eLi, eLp = f32t("sg"), f32t("Lc"), f32t("Lex"), f32t("eL"), f32t("eLi"), f32t("eLp")
    aicl, kk, kk2, rs, kkn, tmpk, kmod = f32t("aicl"), f32t("kk"), f32t("kk2"), f32t("rs"), f32t("kkn"), f32t("tmpk"), f32t("kmod")
    AT, BT, KTt, RT, BpT, KpT, vmb = bft("AT"), bft("BT"), bft("KTt"), bft("RT"), bft("BpT"), bft("KpT"), bft("vmb")
    bv, BTf, KTf, rkk, bonus, gfm = f32t("bv"), f32t("BTf"), f32t("KTf"), f32t("rkk"), f32t("bonus"), f32t("gfm")
    Z = A.alloc("Z", [8, 128], BF16, parts=64)
    Vtm, Bptm, Kptm = bft("Vtm"), bft("Bptm"), bft("Kptm")
    Pb = [[bft(f"Pb{i}"), bft(f"PbT{i}")] for i in range(2)]
    AKT, RBT, RKT = bft("AKT"), bft("RBT"), bft("RKT")
    diagW = f32t("diagW")
    MT, Gb, RbT = bft("MT"), bft("Gb"), bft("RbT")
    YbT, yy, ysq, msq, var, yc = f32t("YbT"), f32t("yy"), f32t("ysq"), f32t("msq"), f32t("var"), f32t("yc")
    oab = A.alloc("oab", [2, 512], BF16, parts=64)

    def v3(ap):
        return ap.rearrange("p (c t) -> p c t", c=8)

    rb = [3]

    def nb():
        b = rb[0]
        rb[0] = 3 + (rb[0] - 3 + 1) % 5
        return b

    mu = lambda q: pv[0:64, q:q + 1]
    mu1 = lambda q: pv1[0:64, q:q + 1]
    W0, A0, KK_, KA_, RK_, LNW, LNB = [pv[0:64, i:i + 1] for i in range(6, 13)]
    KA1 = pv1[0:64, 9:10]

    def a1(g):
        s = g % 2
        for tt in range(4):
            t = g * 4 + tt
            xs = t % 3
            P.dma("sp", xt[:, xs, :], x_d[t * 128:(t + 1) * 128, :], w=[("xt", xs)])
            K.rms_T(xt[:, xs, :], [("xt", xs)], xnT[:, s, :, tt * 128:(tt + 1) * 128], [("xnT", s, tt)], bank=0)
        xk = [("xnT", s, tt) for tt in range(4)]
        tsl = slice(g * 512, (g + 1) * 512)
        if g > 0:
            P.pool(lambda e: e.tensor_copy(out=U[:, s, :, 0:1], in_=U[:, 1 - s, :, 512:513]), r=[("U", 1 - s)], w=[("U", s)])
            P.pool(lambda e: e.tensor_copy(out=Ug[:, s, 0:1], in_=Ug[:, 1 - s, 512:513]), r=[("Ug", 1 - s)], w=[("Ug", s)])
        for q in range(9):
            bk = 1 + q % 2
            M = 128 if q == 8 else 64
            c0 = q * 64

            def mm(e, bk=bk, M=M, c0=c0):
                for j in range(8):
                    ins = e.matmul(K.psf(bk)[0:M, :], lhsT=WA[:, j, c0:c0 + M], rhs=xnT[:, s, j, :], start=(j == 0), stop=(j == 7))
                return ins
            P.pe(mm, r=["WA"] + xk, w=[("ps", bk)])
            src = K.psf(bk)[0:M, :]
            if q < 5:
                P.act(lambda e, q=q, src=src: e.copy(out=U[:, s, q, 1:513], in_=src), r=[("ps", bk)], w=[("U", s)])
            elif q == 5:
                P.act(lambda e, src=src: e.activation(out=qsb[:, s, :], in_=src, func=AF.Copy, scale=0.125), r=[("ps", bk)], w=[("qsb", s)])
                P.dma("sp", QS_d[:, tsl], qsb[:, s, :], r=[("qsb", s)], w=[("QS", g)])
            elif q == 6:
                P.dve(lambda e, src=src: e.tensor_copy(out=ksb[:, s, :], in_=src), r=[("ps", bk)], w=[("ksb", s)])
                P.dve(lambda e, src=src: e.tensor_reduce(out=kms_out[:, 2 * g:2 * g + 2], in_=src.rearrange("p (b k) -> p b k", b=2), axis=AX.X, op=ALU.add),
                      r=[("ps", bk)], w=[("kms", g)])
                P.dma("sp", KS_d[:, tsl], ksb[:, s, :], r=[("ksb", s)], w=[("KS", g)])
            elif q == 7:
                P.act(lambda e, src=src: e.copy(out=mvb, in_=src), r=[("ps", bk)], w=["mvb"])
                pb = K.psb(0)

                def trv(e, pb=pb):
                    for tt in range(4):
                        ins = e.transpose(out=pb[:, tt * 64:(tt + 1) * 64], in_=mvb[:, tt * 128:(tt + 1) * 128], identity=K.ident[0:64, 0:64])
                    return ins
                P.pe(trv, r=["mvb", "ident"], w=[("ps", 0)])
                P.dve(lambda e, pb=pb: e.tensor_copy(out=vsb[:, s, :, :], in_=pb[:, 0:256].rearrange("p (t d) -> p t d", t=4)), r=[("ps", 0)], w=[("vsb", s)])
                P.dma("sp", VS_d[:, 4 * g:4 * g + 4, :], vsb[:, s, :, :], r=[("vsb", s)], w=[("VS", g)])
            else:
                P.act(lambda e, src=src: e.copy(out=Ug[:, s, 1:513], in_=src), r=[("ps", bk)], w=[("Ug", s)])

    def rw(g):
        s = g % 2
        uk = [("U", s)]
        cur = lambda q: U[:, s, q, 1:513]
        prv = lambda q: U[:, s, q, 0:512]

        def mix(q, out, okey, eng_a="pool"):
            P.add(eng_a, lambda e: e.tensor_scalar(out=t1, in0=prv(q), scalar1=mu(q), scalar2=None, op0=ALU.mult), uk + ["pv"], ["t1"])
            P.dve(lambda e: e.scalar_tensor_tensor(out=out, in0=cur(q), scalar=mu1(q), in1=t1, op0=ALU.mult, op1=ALU.add), r=uk + ["pv1", "t1"], w=[okey])
        mix(0, rm, "rm")
        mix(1, km, "km")
        mix(2, vm, "vm")
        mix(3, sg, "sg")
        P.act(lambda e: e.activation(out=tw, in_=sg, func=AF.Tanh), r=["sg"], w=["tw"])
        mix(4, Lex, "Lex")
        P.act(lambda e: e.copy(out=adb, in_=Lex), r=["Lex"], w=["adb"])
        P.pool(lambda e: e.tensor_scalar(out=gdt[:, 0, :], in0=Ug[:, s, 0:512], scalar1=pv[:, 5:6], scalar2=None, op0=ALU.mult), r=[("Ug", s), "pv"], w=[("gdt", 0)])
        P.dve(lambda e: e.scalar_tensor_tensor(out=gdt[:, 1, :], in0=Ug[:, s, 1:513], scalar=pv1[:, 5:6], in1=gdt[:, 0, :], op0=ALU.mult, op1=ALU.add),
              r=[("Ug", s), "pv1", ("gdt", 0)], w=[("gdt", 1)])
        P.act(lambda e: e.activation(out=sgd, in_=gdt[:, 1, :], func=AF.Sigmoid), r=[("gdt", 1)], w=["sgd"])
        b = nb()
        P.pe(lambda e, b=b: e.matmul(K.psf(b)[0:64, :], lhsT=dup, rhs=tw, start=True, stop=True), r=["dup", "tw"], w=[("ps", b)])
        P.act(lambda e, b=b: e.activation(out=sg, in_=K.psf(b)[0:64, :], func=AF.Sigmoid, bias=W0), r=[("ps", b), "pv"], w=["sg"])
        P.dve(lambda e: e.tensor_tensor_scan(out=Lc, data0=resetm, data1=sg, initial=0.0, op0=ALU.mult, op1=ALU.add), r=["cf", "sg"], w=["Lc"])
        P.pool(lambda e: e.tensor_tensor(out=Lex, in0=Lc, in1=sg, op=ALU.subtract), r=["Lc", "sg"], w=["Lex"])
        P.act(lambda e: e.activation(out=eL, in_=Lc, func=AF.Exp, scale=-C0), r=["Lc"], w=["eL"])
        P.act(lambda e: e.activation(out=eLi, in_=Lc, func=AF.Exp, scale=C0), r=["Lc"], w=["eLi"])
        P.act(lambda e: e.activation(out=eLp, in_=Lex, func=AF.Exp, scale=-C0), r=["Lex"], w=["eLp"])
        b = nb()
        P.pe(lambda e, b=b: e.matmul(K.psf(b)[0:64, :], lhsT=iup, rhs=adb, start=True, stop=True), r=["iup", "adb"], w=[("ps", b)])
        P.act(lambda e, b=b: e.activation(out=aicl, in_=K.psf(b)[0:64, :], func=AF.Sigmoid, bias=A0), r=[("ps", b), "pv"], w=["aicl"])
        b = nb()
        P.pe(lambda e, b=b: e.matmul(K.psf(b)[0:64, :], lhsT=gup, rhs=sgd, start=True, stop=True), r=["gup", "sgd"], w=[("ps", b)])
        P.act(lambda e, b=b: e.copy(out=gfm, in_=K.psf(b)[0:64, :]), r=[("ps", b)], w=["gfm"])
        P.pool(lambda e: e.tensor_scalar(out=kk, in0=km, scalar1=KK_, scalar2=None, op0=ALU.mult), r=["km", "pv"], w=["kk"])
        P.pool(lambda e: e.tensor_tensor(out=kk2, in0=kk, in1=kk, op=ALU.mult), r=["kk"], w=["kk2"])
        b = nb()
        P.pe(lambda e, b=b: e.matmul(K.psf(b)[0:64, :], lhsT=ones64f, rhs=kk2, start=True, stop=True), r=["ones64f", "kk2"], w=[("ps", b)])
        P.act(lambda e, b=b: e.activation(out=rs, in_=K.psf(b)[0:64, :], func=AF.Sqrt, bias=eps12[:, 0:1]), r=[("ps", b), "eps12"], w=["rs"])
        P.dve(lambda e: e.reciprocal(out=rs, in_=rs), r=["rs"], w=["rs"])
        P.pool(lambda e: e.tensor_tensor(out=kkn, in0=kk, in1=rs, op=ALU.mult), r=["kk", "rs"], w=["kkn"])
        P.dve(lambda e: e.tensor_scalar(out=tmpk, in0=aicl, scalar1=KA_, scalar2=KA1, op0=ALU.mult, op1=ALU.add), r=["aicl", "pv", "pv1"], w=["tmpk"])
        P.pool(lambda e: e.tensor_tensor(out=kmod, in0=km, in1=tmpk, op=ALU.mult), r=["km", "tmpk"], w=["kmod"])
        P.dve(lambda e: e.scalar_tensor_tensor(out=AT, in0=kkn, scalar=-1.0, in1=eLp, op0=ALU.mult, op1=ALU.mult), r=["kkn", "eLp"], w=["AT"])
        P.pool(lambda e: e.tensor_tensor(out=bv, in0=kkn, in1=aicl, op=ALU.mult), r=["kkn", "aicl"], w=["bv"])
        P.dve(lambda e: e.tensor_tensor(out=BTf, in0=bv, in1=eLi, op=ALU.mult), r=["bv", "eLi"], w=["BTf"])
        P.act(lambda e: e.copy(out=BT, in_=BTf), r=["BTf"], w=["BT"])
        P.pool(lambda e: e.tensor_tensor(out=KTf, in0=kmod, in1=eLi, op=ALU.mult), r=["kmod", "eLi"], w=["KTf"])
        P.act(lambda e: e.copy(out=KTt, in_=KTf), r=["KTf"], w=["KTt"])
        P.dve(lambda e: e.tensor_tensor(out=RT, in0=rm, in1=eL, op=ALU.mult), r=["rm", "eL"], w=["RT"])
        WCb = v3(eL)[:, :, 63:64].to_broadcast([64, 8, 64])
        P.dve(lambda e: e.tensor_tensor(out=v3(BpT), in0=v3(BTf), in1=WCb, op=ALU.mult), r=["BTf", "eL"], w=["BpT"])
        P.pool(lambda e: e.tensor_tensor(out=v3(KpT), in0=v3(KTf), in1=WCb, op=ALU.mult), r=["KTf", "eL"], w=["KpT"])
        P.pool(lambda e: e.tensor_tensor(out=v3(diagW), in0=v3(identrep), in1=WCb, op=ALU.mult), r=["cf", "eL"], w=["diagW"])
        P.act(lambda e: e.copy(out=vmb, in_=vm), r=["vm"], w=["vmb"])
        P.dve(lambda e: e.scalar_tensor_tensor(out=rkk, in0=rm, scalar=RK_, in1=kmod, op0=ALU.mult, op1=ALU.mult), r=["rm", "pv", "kmod"], w=["rkk"])
        b = nb()
        P.pe(lambda e, b=b: e.matmul(K.psf(b)[0:64, :], lhsT=ones64f, rhs=rkk, start=True, stop=True), r=["ones64f", "rkk"], w=[("ps", b)])
        P.dve(lambda e, b=b: e.tensor_tensor(out=bonus, in0=K.psf(b)[0:64, :], in1=vm, op=ALU.mult), r=[("ps", b), "vm"], w=["bonus"])
        id64 = K.ident[0:64, 0:64]
        for (src, skey, dst, dkey) in ((AT, "AT", Z[:, :, 0:64], ("Z", "a")), (vmb, "vmb", v3(Vtm), "Vtm"), (BpT, "BpT", v3(Bptm), "Bptm"), (KpT, "KpT", v3(Kptm), "Kptm")):
            b = nb()
            pb = K.psb(b)[0:64, 0:512]

            def trs(e, src=src, pb=pb):
                for c in range(8):
                    ins = e.transpose(out=pb[:, c * 64:(c + 1) * 64], in_=src[:, c * 64:(c + 1) * 64], identity=id64)
                return ins
            P.pe(trs, r=[skey, "ident"], w=[("ps", b)])
            P.act(lambda e, pb=pb, dst=dst: e.copy(out=dst, in_=v3(pb)), r=[("ps", b)], w=[dkey])
        M_LS, M_US, M_UI = msk[:, 0, :], msk[:, 1, :], msk[:, 2, :]
        Pc, PcT = Pb[0]
        for (lh, lk, rh, rk_, dst, dkey, m_) in ((AT, "AT", BT, "BT", Pc, "Pb0", M_LS), (BT, "BT", AT, "AT", PcT, "PbT0", M_US),
                                                 (KTt, "KTt", AT, "AT", AKT, "AKT", M_US), (BT, "BT", RT, "RT", RBT, "RBT", M_UI),
                                                 (KTt, "KTt", RT, "RT", RKT, "RKT", M_UI)):
            b = nb()

            def sc(e, lh=lh, rh=rh, b=b):
                for c in range(8):
                    ins = e.matmul(K.psf(b)[0:64, c * 64:(c + 1) * 64], lhsT=lh[:, c * 64:(c + 1) * 64], rhs=rh[:, c * 64:(c + 1) * 64], start=True, stop=True)
                return ins
            P.pe(sc, r=[lk, rk_], w=[("ps", b)])
            P.dve(lambda e, b=b, dst=dst, m_=m_: e.tensor_tensor(out=dst, in0=K.psf(b)[0:64, :], in1=m_, op=ALU.mult), r=[("ps", b), "msk"], w=[dkey])
        b = nb()

        def x1(e, b=b):
            for c in range(8):
                ins = e.matmul(K.psf(b)[0:64, c * 64:(c + 1) * 64], lhsT=AKT[:, c * 64:(c + 1) * 64], rhs=Vtm[:, c * 64:(c + 1) * 64], start=True, stop=True)
            return ins
        P.pe(x1, r=["AKT", "Vtm"], w=[("ps", b)])
        P.act(lambda e, b=b: e.copy(out=Z[:, :, 64:128], in_=v3(K.psf(b)[0:64, :])), r=[("ps", b)], w=[("Z", "x")])
        zk = [("Z", "a"), ("Z", "x")]
        for lvl in range(6):
            Pc, PcT = Pb[lvl % 2]
            pk, ptk = f"Pb{lvl % 2}", f"PbT{lvl % 2}"
            b1, b2 = nb(), nb()

            def app(e, PcT=PcT, b1=b1, b2=b2):
                for c in range(8):
                    bb = b1 if c < 4 else b2
                    cc = c % 4
                    ins = e.matmul(K.psf(bb)[0:64, cc * 128:(cc + 1) * 128], lhsT=PcT[:, c * 64:(c + 1) * 64], rhs=Z[:, c, :], start=True, stop=True)
                return ins
            P.pe(app, r=[ptk] + zk, w=[("ps", b1), ("ps", b2)])
            P.dve(lambda e, b1=b1: e.tensor_tensor(out=Z[:, 0:4, :], in0=K.psf(b1)[0:64, :].rearrange("p (c k) -> p c k", c=4), in1=Z[:, 0:4, :], op=ALU.add),
                  r=[("ps", b1)] + zk, w=zk)
            P.dve(lambda e, b2=b2: e.tensor_tensor(out=Z[:, 4:8, :], in0=K.psf(b2)[0:64, :].rearrange("p (c k) -> p c k", c=4), in1=Z[:, 4:8, :], op=ALU.add),
                  r=[("ps", b2)] + zk, w=zk)
            if lvl < 5:
                Pn, PnT = Pb[(lvl + 1) % 2]
                nk, ntk = f"Pb{(lvl + 1) % 2}", f"PbT{(lvl + 1) % 2}"
                b1, b2 = nb(), nb()

                def sq(e, Pc=Pc, PcT=PcT, b1=b1, b2=b2):
                    for c in range(8):
                        cs = slice(c * 64, (c + 1) * 64)
                        e.matmul(K.psf(b1)[0:64, cs], lhsT=PcT[:, cs], rhs=Pc[:, cs], start=True, stop=True)
                        ins = e.matmul(K.psf(b2)[0:64, cs], lhsT=Pc[:, cs], rhs=PcT[:, cs], start=True, stop=True)
                    return ins
                P.pe(sq, r=[pk, ptk], w=[("ps", b1), ("ps", b2)])
                P.act(lambda e, b1=b1, Pn=Pn: e.copy(out=Pn, in_=K.psf(b1)[0:64, :]), r=[("ps", b1)], w=[nk])
                P.dve(lambda e, b2=b2, PnT=PnT: e.tensor_copy(out=PnT, in_=K.psf(b2)[0:64, :]), r=[("ps", b2)], w=[ntk])
        cs_ = lambda c: slice(c * 64, (c + 1) * 64)
        b = nb()

        def dMT(e, b=b):
            for c in range(8):
                ins = e.matmul(K.psf(b)[0:64, cs_(c)], lhsT=Z[:, c, 0:64], rhs=Bptm[:, cs_(c)], start=True, stop=True)
            return ins
        P.pe(dMT, r=zk + ["Bptm"], w=[("ps", b)])
        P.dve(lambda e, b=b: e.tensor_tensor(out=MT, in0=K.psf(b)[0:64, :], in1=diagW, op=ALU.add), r=[("ps", b), "diagW"], w=["MT"])
        b = nb()

        def dG(e, b=b):
            for c in range(8):
                e.matmul(K.psf(b)[0:64, cs_(c)], lhsT=Bptm[:, cs_(c)], rhs=Z[:, c, 64:128], start=True, stop=False)
                ins = e.matmul(K.psf(b)[0:64, cs_(c)], lhsT=Kptm[:, cs_(c)], rhs=Vtm[:, cs_(c)], start=False, stop=True)
            return ins
        P.pe(dG, r=zk + ["Bptm", "Kptm", "Vtm"], w=[("ps", b)])
        P.act(lambda e, b=b: e.copy(out=Gb, in_=K.psf(b)[0:64, :]), r=[("ps", b)], w=["Gb"])
        b = nb()

        def dR(e, b=b):
            for c in range(8):
                ins = e.matmul(K.psf(b)[0:64, cs_(c)], lhsT=Z[:, c, 0:64], rhs=RBT[:, cs_(c)], start=True, stop=True)
            return ins
        P.pe(dR, r=zk + ["RBT"], w=[("ps", b)])
        P.dve(lambda e, b=b: e.tensor_tensor(out=RbT, in0=K.psf(b)[0:64, :], in1=RT, op=ALU.add), r=[("ps", b), "RT"], w=["RbT"])
        b = nb()

        def dY(e, b=b):
            for c in range(8):
                e.matmul(K.psf(b)[0:64, cs_(c)], lhsT=Z[:, c, 64:128], rhs=RBT[:, cs_(c)], start=True, stop=False)
                ins = e.matmul(K.psf(b)[0:64, cs_(c)], lhsT=Vtm[:, cs_(c)], rhs=RKT[:, cs_(c)], start=False, stop=True)
            return ins
        P.pe(dY, r=zk + ["RBT", "RKT", "Vtm"], w=[("ps", b)])
        P.act(lambda e, b=b: e.copy(out=YbT, in_=K.psf(b)[0:64, :]), r=[("ps", b)], w=["YbT"])
        for c in range(8):
            b = nb()

            def rec(e, b=b, c=c):
                e.matmul(K.psf(b)[0:64, 0:64], lhsT=id64, rhs=Gb[:, cs_(c)], start=True, stop=False)
                return e.matmul(K.psf(b)[0:64, 0:64], lhsT=MT[:, cs_(c)], rhs=Hs[:, s, c, :], start=False, stop=True)
            P.pe(rec, r=["ident", "Gb", "MT", ("Hs", s, c)], w=[("ps", b)])
            if c < 7:
                dst, dk = Hs[:, s, c + 1, :], ("Hs", s, c + 1)
            else:
                dst, dk = Hs[:, 1 - s, 0, :], ("Hs", 1 - s, 0)
            P.act(lambda e, b=b, dst=dst: e.copy(out=dst, in_=K.psf(b)[0:64, 0:64]), r=[("ps", b)], w=[dk])
        b = nb()

        def oy(e, b=b):
            for c in range(8):
                ins = e.matmul(K.psf(b)[0:64, cs_(c)], lhsT=Hs[:, s, c, :], rhs=RbT[:, cs_(c)], start=True, stop=True)
            return ins
        P.pe(oy, r=[("Hs", s, c) for c in range(8)] + ["RbT"], w=[("ps", b)])
        P.dve(lambda e, b=b: e.tensor_tensor(out=yy, in0=K.psf(b)[0:64, :], in1=YbT, op=ALU.add), r=[("ps", b), "YbT"], w=["yy"])
        P.act(lambda e: e.activation(out=ysq, in_=yy, func=AF.Square), r=["yy"], w=["ysq"])
        bm, bq = nb(), nb()
        P.pe(lambda e, bm=bm: e.matmul(K.psf(bm)[0:64, :], lhsT=ones64f, rhs=yy, start=True, stop=True), r=["ones64f", "yy"], w=[("ps", bm)])
        P.pe(lambda e, bq=bq: e.matmul(K.psf(bq)[0:64, :], lhsT=ones64f, rhs=ysq, start=True, stop=True), r=["ones64f", "ysq"], w=[("ps", bq)])
        P.act(lambda e, bm=bm: e.activation(out=msq, in_=K.psf(bm)[0:64, :], func=AF.Square, scale=1.0 / 64), r=[("ps", bm)], w=["msq"])
        P.dve(lambda e, bq=bq: e.scalar_tensor_tensor(out=var, in0=K.psf(bq)[0:64, :], scalar=1.0 / 64, in1=msq, op0=ALU.mult, op1=ALU.subtract), r=[("ps", bq), "msq"], w=["var"])
        P.act(lambda e: e.activation(out=var, in_=var, func=AF.Sqrt, bias=eps12[:, 1:2]), r=["var", "eps12"], w=["var"])
        P.dve(lambda e: e.reciprocal(out=var, in_=var), r=["var"], w=["var"])
        P.dve(lambda e, bm=bm: e.scalar_tensor_tensor(out=yc, in0=K.psf(bm)[0:64, :], scalar=-1.0 / 64, in1=yy, op0=ALU.mult, op1=ALU.add), r=[("ps", bm), "yy"], w=["yc"])
        P.pool(lambda e: e.tensor_tensor(out=yc, in0=yc, in1=var, op=ALU.mult), r=["yc", "var"], w=["yc"])
        P.act(lambda e: e.activation(out=yc, in_=yc, func=AF.Identity, scale=LNW, bias=LNB), r=["yc", "pv"], w=["yc"])
        P.pool(lambda e: e.tensor_tensor(out=yc, in0=yc, in1=bonus, op=ALU.add), r=["yc", "bonus"], w=["yc"])
        P.dve(lambda e: e.tensor_tensor(out=oab[:, s, :], in0=yc, in1=gfm, op=ALU.mult), r=["yc", "gfm"], w=[("oab", s)])
        j, off = g // 4, (g % 4) * 512
        P.dma("sp", XI_d[j, 0:64, off:off + 512], oab[:, s, :], r=[("oab", s)], w=[("XI", g)])
        if dbg is not None and "oa" in dbg:
            P.dma("sp", dbg["oa"][:, g * 512:(g + 1) * 512], oab[:, s, :], r=[("oab", s)])

    gdt = A.alloc("gdt", [2, 512], F32)
    for g in range(ngroups + 1):
        if g < ngroups:
            a1(g)
        if g >= 1 and g - 1 < nrw:
            rw(g - 1)


LEXT = 4096
MNEAR = 23


def phase_moba(K, XI_d, QS_d, KS_d, VS_d, kms, ext_t, dbg=None, nq=32):
    nc, P, A = K.nc, K.P, K.A
    rb_d = K.din("rbh", [32, 1])
    oh_d = K.din("c_onehot", [33, LEXT])
    ce_d = K.din("c_eind", [64, T])
    cm_d = K.din("c_mbmask", [128, 3, 128])
    aid_d = K.din("c_antiid", [128, 128])
    NGR = T // 512
    KA = A.alloc("KA", [T], BF16)
    QA = A.alloc("QA", [T], BF16)
    VA = A.alloc("VA", [128, 66], BF16)
    Toep = A.alloc("Toep", [MNEAR + 4, 512], BF16)
    PT = A.alloc("PTm", [3, 512], BF16)
    cmk = A.alloc("cmk", [3, 128], F32)
    kmT = A.alloc("kmT", [64], BF16, parts=64)
    smt = A.alloc("smt", [64], F32)
    val = A.alloc("valm", [64], F32)
    top8 = A.alloc("top8m", [8], F32)
    MBpad = A.alloc("MBpad", [4, 128], BF16)
    b31 = A.alloc("b31", [1], F32)
    rbx = A.alloc("rbx", [1], F32, parts=64)
    onesf = A.alloc("onesf", [64], F32)
    den = A.alloc("den", [512], F32)
    dbc = A.alloc("dbc", [512], F32, parts=64)
    obb = A.alloc("obb", [2, 512], BF16, parts=64)
    extsb = A.alloc("extsb", [LEXT], BF16, parts=1)
    antiid = A.alloc("antiid", [128], BF16)
    P.dma("pool", antiid, aid_d, w=["antiid"])
    allg = range(NGR)
    P.dma("sp", KA[0:64, :], KS_d, r=[("KS", g) for g in allg], w=["KA"])
    P.dma("pool", KA[64:128, :], ce_d, w=["KA"])
    P.dma("sp", QA[0:64, :], QS_d, r=[("QS", g) for g in allg], w=["QA"])
    P.dma("sp", VA[:, :, 0:64], VS_d, r=[("VS", g) for g in allg], w=["VA"])
    P.dve(lambda e: e.memset(VA[:, :, 64:65], 1.0), w=["VA"])
    P.dve(lambda e: e.memset(MBpad[:, :, 0:64], 0.0), w=["MBpad"])
    P.dve(lambda e: e.memset(onesf, 1.0), w=["onesf"])
    P.dma("sp", cmk, cm_d, w=["cmk"])
    P.act(lambda e: e.copy(out=kmT, in_=kms), r=[("kms", g) for g in allg], w=["kmT"])
    P.dma("sp", b31, rb_d[31:32, 0:1].partition_broadcast(128), w=["b31"])
    P.dma("sp", rbx[0:32, :], rb_d, w=["rbx"])
    P.dve(lambda e: e.memset(rbx[32:33, :], -30000.0), w=["rbx"])
    mk = A.mark()
    OH = A.alloc("OH", [LEXT], F32, parts=64)
    P.dma("sp", OH[0:33, :], oh_d, w=["OH"])
    for n in range(LEXT // 512):
        bk = 1 + n % 3
        P.pe(lambda e, n=n, bk=bk: e.matmul(K.psf(bk)[0:1, :], lhsT=rbx[0:33, :], rhs=OH[0:33, n * 512:(n + 1) * 512], start=True, stop=True), r=["rbx", "OH"], w=[("ps", bk)])
        P.act(lambda e, n=n, bk=bk: e.copy(out=extsb[:, n * 512:(n + 1) * 512], in_=K.psf(bk)[0:1, :]), r=[("ps", bk)], w=["extsb"])
    ext_ap = bass.AP(ext_t, 0, [[LEXT, 1], [1, LEXT]])
    P.dma("sp", ext_ap, extsb, r=["extsb"], w=["ext"])
    for m in range(-3, MNEAR + 1):
        P.dma("sp", Toep[:, m + 3, :], bass.AP(ext_t, 384 + 128 * m, [[1, 128], [1, 512]]), r=["ext"], w=["Toep"])
    A.release(mk)
    PAST, CM2, SMK = cmk[:, 0, :], cmk[:, 1, :], cmk[:, 2, :]

    for qi in range(nq):
        q0 = qi * 512
        qsl = slice(q0, q0 + 512)

        def sc(e, q0=q0):
            for st in range(4):
                ins = e.matmul(K.psf(0)[:, st * 64:(st + 1) * 64], lhsT=QA[0:64, q0 + st * 128:q0 + (st + 1) * 128], rhs=kmT, start=True, stop=True)
            return ins
        P.pe(sc, r=["QA", "kmT"], w=[("ps", 0)])
        for st in range(4):
            qb = 2 * qi + st // 2
            wsl = slice(64 - qb, 128 - qb)
            P.dve(lambda e, st=st, wsl=wsl: e.tensor_tensor(out=smt, in0=K.psf(0)[:, st * 64:(st + 1) * 64], in1=SMK[:, wsl], op=ALU.add), r=[("ps", 0), "cmk"], w=["smt"])
            P.dve(lambda e: e.max(out=top8, in_=smt), r=["smt"], w=["top8m"])
            P.dve(lambda e: e.tensor_scalar(out=val, in0=smt, scalar1=top8[:, 2:3], scalar2=30000.0, op0=ALU.is_ge, op1=ALU.mult), r=["smt", "top8m"], w=["valm"])
            P.dve(lambda e, wsl=wsl: e.tensor_tensor(out=val, in0=val, in1=PAST[:, wsl], op=ALU.mult), r=["valm", "cmk"], w=["valm"])
            P.dve(lambda e, st=st, wsl=wsl: e.tensor_tensor(out=MBpad[:, st, 64:128], in0=val, in1=CM2[:, wsl], op=ALU.add), r=["valm", "cmk"], w=[("MBpad", st)])
        pb = K.psb(7)

        def trm(e, pb=pb):
            for st in range(4):
                ins = e.transpose(out=pb[:, st * 128:(st + 1) * 128], in_=MBpad[:, st, :], identity=K.ident)
            return ins
        P.pe(trm, r=[("MBpad", st) for st in range(4)] + ["MBpad", "ident"], w=[("ps", 7)])
        P.act(lambda e, pb=pb, qsl=qsl: e.copy(out=QA[64:128, qsl], in_=pb[64:128, 0:512]), r=[("ps", 7)], w=[("QAm", qi)])
        nk = 4 * qi + 4
        pend = None
        for kt in range(nk):
            m = 4 * qi - kt
            bk = 1 + kt % 3
            slot = kt % 3
            near = m <= MNEAR

            def lg(e, kt=kt, bk=bk, m=m, near=near, qsl=qsl):
                ins = e.matmul(K.psf(bk), lhsT=KA[:, kt * 128:(kt + 1) * 128], rhs=QA[:, qsl], start=True, stop=not near)
                if near:
                    ins = e.matmul(K.psf(bk), lhsT=antiid, rhs=Toep[:, m + 3, :], start=False, stop=True)
                return ins
            P.pe(lg, r=["KA", "QA", ("QAm", qi), "Toep", "antiid"], w=[("ps", bk)])
            if near:
                P.act(lambda e, bk=bk, slot=slot: e.activation(out=PT[:, slot, :], in_=K.psf(bk), func=AF.Exp), r=[("ps", bk)], w=[("PTm", slot)])
            else:
                P.act(lambda e, bk=bk, slot=slot: e.activation(out=PT[:, slot, :], in_=K.psf(bk), func=AF.Exp, bias=b31[:, 0:1]), r=[("ps", bk), "b31"], w=[("PTm", slot)])
            if pend is not None:
                pend()
            pend = (lambda kt=kt, slot=slot, nk=nk: P.pe(lambda e: e.matmul(K.psf(4)[0:65, :], lhsT=VA[:, kt, 0:65], rhs=PT[:, slot, :], start=(kt == 0), stop=(kt == nk - 1)),
                                                          r=["VA", ("PTm", slot)], w=[("ps", 4)]))
        pend()
        s = qi % 2
        P.act(lambda e: e.copy(out=den[64:65, :], in_=K.psf(4)[64:65, :]), r=[("ps", 4)], w=["den"])
        P.dve(lambda e: e.reciprocal(out=den[64:65, :], in_=den[64:65, :]), r=["den"], w=["den"])
        P.pe(lambda e: e.matmul(K.psf(5)[0:64, :], lhsT=onesf[64:65, :], rhs=den[64:65, :], start=True, stop=True), r=["onesf", "den"], w=[("ps", 5)])
        P.act(lambda e: e.copy(out=dbc, in_=K.psf(5)[0:64, :]), r=[("ps", 5)], w=["dbc"])
        P.dve(lambda e, s=s: e.tensor_tensor(out=obb[:, s, :], in0=K.psf(4)[0:64, :], in1=dbc, op=ALU.mult), r=[("ps", 4), "dbc"], w=[("obb", s)])
        j, off = qi // 4, (qi % 4) * 512
        P.dma("sp", XI_d[j, 64:128, off:off + 512], obb[:, s, :], r=[("obb", s)], w=[("XIb", qi)])
        if dbg is not None and "ob" in dbg:
            P.dma("sp", dbg["ob"][:, qsl], obb[:, s, :], r=[("obb", s)])


def _t5_bucket_table(n):
    import jax
    import jax.numpy as jnp
    with jax.default_device(jax.devices("cpu")[0]):
        d = jnp.arange(n)
        nn = jnp.maximum(d, 0)
        nf = jnp.maximum(nn, 16).astype(jnp.float32)
        large = 16 + (jnp.log(nf / 16) / math.log(4096 / 16) * 16).astype(jnp.int32)
        large = jnp.minimum(large, 31)
        return np.asarray(jnp.where(nn < 16, nn, large))


_CONST = {}


def host_constants():
    if _CONST:
        return _CONST
    c = {}
    c["c_ident"] = np.eye(128, dtype=np.float32)
    c["c_antiid"] = np.ascontiguousarray(np.eye(128, dtype=np.float32)[::-1])
    r = np.arange(64)[:, None]
    col = np.arange(64)[None, :]
    crw = np.zeros((64, 5, 512), np.float32)
    crw[:, 0] = np.tile((col < r).astype(np.float32), (1, 8))
    crw[:, 1] = np.tile((r < col).astype(np.float32), (1, 8))
    crw[:, 2] = np.tile((r <= col).astype(np.float32), (1, 8))
    crw[:, 3] = (np.arange(512) % 64 != 0).astype(np.float32)[None, :]
    crw[:, 4] = np.tile(np.eye(64, dtype=np.float32), (1, 8))
    c["c_rwkv"] = crw
    bt = _t5_bucket_table(LEXT)
    oh = np.zeros((33, LEXT), np.float32)
    for x in range(LEXT):
        dist = x - 511
        if dist < 0:
            oh[32, x] = 1.0
        else:
            oh[bt[dist], x] = 1.0
    c["c_onehot"] = oh
    c["c_eind"] = (np.arange(T)[None, :] // 256 == np.arange(64)[:, None]).astype(np.float32)
    j = np.arange(128)
    cm = np.zeros((128, 3, 128), np.float32)
    cm[:, 0] = (j < 64).astype(np.float32)[None, :]
    cm[:, 1] = np.where(j == 64, 0.0, -30000.0).astype(np.float32)[None, :]
    cm[:, 2] = np.where(j < 64, 0.0, -1e30).astype(np.float32)[None, :]
    c["c_mbmask"] = cm
    _CONST.update(c)
    return _CONST


def core_inputs(inp, h):
    w_in = inp["w_in"][0]
    cols = np.concatenate([np.arange(64 * h, 64 * h + 64), 512 + np.arange(64 * h, 64 * h + 64), 1024 + np.arange(64 * h, 64 * h + 64),
                           np.arange(1536, 1600), np.arange(1600, 1664),
                           1792 + np.arange(64 * h, 64 * h + 64), 2304 + np.arange(64 * h, 64 * h + 64), 2816 + np.arange(64 * h, 64 * h + 64),
                           np.arange(1664, 1792)])
    mu = inp["tshift_mu"][0]
    hs = slice(64 * h, 64 * h + 64)
    pvec = np.zeros((128, 16), np.float32)
    pvec[0:64, 0] = mu[hs]
    pvec[0:64, 1] = mu[512 + 64 * h:512 + 64 * h + 64]
    pvec[0:64, 2] = mu[1024 + 64 * h:1024 + 64 * h + 64]
    pvec[0:64, 3] = mu[1536:1600]
    pvec[0:64, 4] = mu[1600:1664]
    pvec[:, 5] = mu[1664:1792]
    pvec[0:64, 6] = inp["decay_w0"][0][hs]
    pvec[0:64, 7] = inp["iclr_a0"][0][hs]
    pvec[0:64, 8] = inp["k_k"][0][hs]
    pvec[0:64, 9] = inp["k_a"][0][hs]
    pvec[0:64, 10] = inp["r_k"][0][h]
    pvec[0:64, 11] = inp["ln_x_w"][0][hs]
    pvec[0:64, 12] = inp["ln_x_b"][0][hs]
    sl = slice(h * TOWN, (h + 1) * TOWN)
    m = dict(host_constants())
    m.update({
        "x": inp["x"][0], "xo": np.ascontiguousarray(inp["x"][0, sl]),
        "wa": np.ascontiguousarray(w_in[:, cols]), "pvec": pvec,
        "decay_up_h": np.ascontiguousarray(inp["decay_up"][0][:, hs]), "iclr_up_h": np.ascontiguousarray(inp["iclr_up"][0][:, hs]),
        "gate_up_h": np.ascontiguousarray(inp["gate_up"][0][:, hs]), "rbh": np.ascontiguousarray(inp["rel_bias"][:, h:h + 1]),
        "w_gate": np.ascontiguousarray(w_in[:, 3328:]), "norm_mix": inp["norm_mix"][0],
        "w_o_rwkv": inp["w_o_rwkv"][0], "w_o_moba": inp["w_o_moba"][0], "w_out": inp["w_out"][0],
        "norm_xattn": inp["norm_xattn"][0], "w_q_x": inp["w_q_x"][0], "w_kv_x": inp["w_kv_x"][0], "w_o_x": inp["w_o_x"][0],
        "mem": inp["mem"][0], "mem_norm": inp["mem_norm"], "norm_ffn": inp["norm_ffn"][0],
        "w_router": np.ascontiguousarray(np.concatenate([inp["w_router_group"][0], inp["w_router_expert"][0]], axis=1)),
        "w_exp_gate": inp["w_exp_gate"][0], "w_exp_up": inp["w_exp_up"][0], "w_exp_down": inp["w_exp_down"][0], "norm_final": inp["norm_final"],
    })
    return m


def build_phase_a():
    nc = bass.Bass("TRN2", target_bir_lowering=False)
    with ExitStack() as st:
        K = KB(nc, st)
        K.common()
        x_d = K.din("x", [T, D])
        XI_d = K.dout("XI", [8, 128, TOWN], BF16)
        QS_d = nc.dram_tensor("QS", [64, T], BF16, kind="Internal").ap()
        KS_d = nc.dram_tensor("KS", [64, T], BF16, kind="Internal").ap()
        VS_d = nc.dram_tensor("VS", [128, T // 128, 64], BF16, kind="Internal").ap()
        ext_t = nc.dram_tensor("ext", [LEXT], BF16, kind="Internal")
        kms = K.A.alloc("kms", [64], F32, parts=64)
        mk = K.A.mark()
        phase_a1_rwkv(K, x_d, XI_d, QS_d, KS_d, VS_d, kms)
        K.A.release(mk)
        phase_moba(K, XI_d, QS_d, KS_d, VS_d, kms, ext_t)
        K.P.emit()
        names = set(K.dram.keys())
    return nc, names


def build_phase_b():
    nc = bass.Bass("TRN2", target_bir_lowering=False)
    with ExitStack() as st:
        K = KB(nc, st)
        K.common()
        G_d = K.din("G", [8, 128, TOWN], BF16)
        out_d = K.dout("out", [TOWN, D])
        xres = phase_b(K, G_d, out_d)
        phase_b_moe(K, xres, out_d)
        K.P.emit()
        names = set(K.dram.keys())
    return nc, names


def kernel_two_launch(inp):
    ncA, namesA = build_phase_a()
    maps = []
    cores = [core_inputs(inp, h) for h in range(NCORES)]
    resA = run_bass_kernel_spmd(ncA, [{k: v for k, v in m.items() if k in namesA} for m in cores], core_ids=list(range(NCORES)))
    XI = np.stack([resA.results[h]["XI"] for h in range(NCORES)])
    ncB, namesB = build_phase_b()
    mapsB = []
    for c in range(NCORES):
        m = {k: v for k, v in cores[c].items() if k in namesB}
        m["G"] = np.ascontiguousarray(XI[:, c])
        mapsB.append(m)
    resB = run_bass_kernel_spmd(ncB, mapsB, core_ids=list(range(NCORES)))
    out = np.concatenate([resB.results[c]["out"] for c in range(NCORES)], axis=0)
    return out.reshape(1, T, D).astype(np.float32)


MODE = "two"


def kernel(**inputs):
    inp = {k: np.asarray(v) for k, v in inputs.items()}
    if MODE == "two":
        return kernel_two_launch(inp)
    return kernel_fused(inp)
```
